# Optimizing a Trainium2 kernel written in Bass

```python
import jax
import jax.numpy as jnp
from jax import lax
import numpy as np

D_MODEL = 2048
BATCH = 1
SEQ = 8192
DEPTH = 4

CHUNK = 64
D_CONV = D_MODEL // 2
D_ATT = D_MODEL // 2
D_MIX = D_CONV + D_ATT
HEAD_DIM = 128
N_HEADS = D_ATT // HEAD_DIM
CONV_WIDTH = 3
Q_BLOCK = 128
N_EXPERTS = 64
TOP_K = 8
N_GROUPS = 8
TOPK_GROUPS = 4
D_EXPERT = 384
ROUTED_SCALE = 2.5
EXPERT_BLOCK = 256
DEEPNORM_ALPHA = (2 * DEPTH) ** 0.25
DEEPNORM_BETA = (8 * DEPTH) ** -0.25
LN_EPS = 1e-5
RMS_EPS = 1e-6
N_MOD = 6

kernel_name = "hybrid_conv_stickbreaking_moe_deepnorm_adaln"


def layer_norm(x, g, b):
    xf = x.astype(jnp.float32)
    mu = jnp.mean(xf, axis=-1, keepdims=True)
    xc = xf - mu
    var = jnp.mean(xc * xc, axis=-1, keepdims=True)
    y = xc * lax.rsqrt(var + LN_EPS) * g.astype(jnp.float32) + b.astype(jnp.float32)
    return y.astype(x.dtype)


def rms_norm(x, g):
    xf = x.astype(jnp.float32)
    y = xf * lax.rsqrt(jnp.mean(xf * xf, axis=-1, keepdims=True) + RMS_EPS) * g.astype(jnp.float32)
    return y.astype(x.dtype)


def short_gated_conv(h, b_gate, c_gate, conv_w):
    s = h.shape[1]
    v = c_gate * h
    vp = jnp.pad(v, ((0, 0), (CONV_WIDTH - 1, 0), (0, 0)))
    conv = sum(vp[:, k:k + s, :] * conv_w[k] for k in range(CONV_WIDTH))
    return b_gate * conv


def stick_breaking_attention(q, k, v):
    bsz, s, _ = q.shape
    n_blk = s // Q_BLOCK
    q = q.reshape(bsz, s, N_HEADS, HEAD_DIM).transpose(0, 2, 1, 3)
    kf = k.reshape(bsz, s, N_HEADS, HEAD_DIM).transpose(0, 2, 1, 3).astype(jnp.float32)
    vf = v.reshape(bsz, s, N_HEADS, HEAD_DIM).transpose(0, 2, 1, 3).astype(jnp.float32)
    q_blocks = q.reshape(bsz, N_HEADS, n_blk, Q_BLOCK, HEAD_DIM).transpose(2, 0, 1, 3, 4)
    s_pos = jnp.arange(s, dtype=jnp.int32)
    scale = HEAD_DIM ** -0.5

    def one_block(args):
        i, qi = args
        z = jnp.einsum('bhqd,bhkd->bhqk', qi.astype(jnp.float32), kf) * scale
        t_pos = i * Q_BLOCK + jnp.arange(Q_BLOCK, dtype=jnp.int32)
        mask = s_pos[None, :] < t_pos[:, None]
        log_keep = jnp.where(mask, jax.nn.log_sigmoid(-z), 0.0)
        later = lax.cumsum(log_keep, axis=3, reverse=True) - log_keep
        a = jnp.where(mask, jnp.exp(jax.nn.log_sigmoid(z) + later), 0.0)
        return jnp.einsum('bhqk,bhkd->bhqd', a, vf)

    o = lax.map(one_block, (jnp.arange(n_blk, dtype=jnp.int32), q_blocks))
    o = o.transpose(1, 0, 3, 2, 4).reshape(bsz, s, D_ATT)
    return o.astype(v.dtype)


def hybrid_mixer(u, w_in, conv_w, g_conv, g_att, w_out):
    proj = jnp.einsum('bsd,de->bse', u, w_in)
    cuts = [D_CONV, 2 * D_CONV, 3 * D_CONV, 3 * D_CONV + D_ATT, 3 * D_CONV + 2 * D_ATT]
    h, b_gate, c_gate, q, k, v = jnp.split(proj, cuts, axis=-1)
    y_conv = short_gated_conv(h, b_gate, c_gate, conv_w)
    y_att = stick_breaking_attention(q, k, v)
    y = jnp.concatenate([rms_norm(y_conv, g_conv), rms_norm(y_att, g_att)], axis=-1)
    return jnp.einsum('bse,ed->bsd', y, w_out)


def swiglu(x, wg, wu, wd):
    return (jax.nn.silu(x @ wg) * (x @ wu)) @ wd


def route(xf, w_router, router_bias):
    t = xf.shape[0]
    scores = jax.nn.sigmoid(xf.astype(jnp.float32) @ w_router.astype(jnp.float32))
    sel = scores + router_bias.astype(jnp.float32)
    grp = sel.reshape(t, N_GROUPS, N_EXPERTS // N_GROUPS)
    group_score = lax.top_k(grp, 2)[0].sum(-1)
    _, gidx = lax.top_k(group_score, TOPK_GROUPS)
    gmask = jax.nn.one_hot(gidx, N_GROUPS, dtype=jnp.float32).sum(-2) > 0
    emask = jnp.repeat(gmask, N_EXPERTS // N_GROUPS, axis=-1)
    _, eidx = lax.top_k(jnp.where(emask, sel, -jnp.inf), TOP_K)
    w = jnp.take_along_axis(scores, eidx, axis=-1)
    w = w / jnp.sum(w, axis=-1, keepdims=True) * ROUTED_SCALE
    return eidx.astype(jnp.int32), w


def moe_ffn(u, w_router, router_bias, w_gate, w_up, w_down, ws_gate, ws_up, ws_down):
    bsz, s, d = u.shape
    t = bsz * s
    xf = u.reshape(t, d)
    eidx, w = route(xf, w_router, router_bias)
    n_assign = t * TOP_K
    flat_e = eidx.reshape(n_assign)
    flat_tok = jnp.repeat(jnp.arange(t, dtype=jnp.int32), TOP_K)
    flat_w = w.reshape(n_assign)
    order = jnp.argsort(flat_e)
    se = flat_e[order]
    counts = jnp.zeros((N_EXPERTS,), jnp.int32).at[flat_e].add(1)
    blocks_per_e = (counts + EXPERT_BLOCK - 1) // EXPERT_BLOCK
    blk_end = jnp.cumsum(blocks_per_e)
    pad_start = (blk_end - blocks_per_e) * EXPERT_BLOCK
    start = jnp.cumsum(counts) - counts
    dest = pad_start[se] + jnp.arange(n_assign, dtype=jnp.int32) - start[se]
    n_blocks = -(-n_assign // EXPERT_BLOCK) + N_EXPERTS
    n_rows = n_blocks * EXPERT_BLOCK
    tok_buf = jnp.zeros((n_rows,), jnp.int32).at[dest].set(flat_tok[order])
    w_buf = jnp.zeros((n_rows,), flat_w.dtype).at[dest].set(flat_w[order])
    blk_expert = jnp.minimum(
        jnp.searchsorted(blk_end, jnp.arange(n_blocks, dtype=jnp.int32), side='right'),
        N_EXPERTS - 1).astype(jnp.int32)

    def expert_block(args):
        tok, e = args
        return swiglu(xf[tok], w_gate[e], w_up[e], w_down[e])

    out = lax.map(expert_block, (tok_buf.reshape(n_blocks, EXPERT_BLOCK), blk_expert))
    out = out.reshape(n_rows, d) * w_buf[:, None].astype(out.dtype)
    routed = jax.ops.segment_sum(out, tok_buf, num_segments=t)
    shared = swiglu(xf, ws_gate, ws_up, ws_down)
    return (routed + shared).reshape(bsz, s, d)


def setup_inputs(seed: int = 0) -> dict:
    key = jax.random.key(seed)
    ks = jax.random.split(key, 24)

    def nrm(k, shape, scale):
        return jax.random.normal(k, shape, jnp.float32) * scale

    d_in = 3 * D_CONV + 3 * D_ATT
    return {
        "x": nrm(ks[0], (BATCH, SEQ, D_MODEL), 1.0),
        "c": nrm(ks[1], (BATCH, D_MODEL), 1.0),
        "w_mod": nrm(ks[2], (DEPTH, D_MODEL, N_MOD * D_MODEL), 0.1 * D_MODEL ** -0.5),
        "b_mod": nrm(ks[3], (DEPTH, N_MOD * D_MODEL), 0.01),
        "w_in": nrm(ks[4], (DEPTH, D_MODEL, d_in), D_MODEL ** -0.5),
        "conv_w": nrm(ks[5], (DEPTH, CONV_WIDTH, D_CONV), CONV_WIDTH ** -0.5),
        "g_conv": 1.0 + nrm(ks[6], (DEPTH, D_CONV), 0.02),
        "g_att": 1.0 + nrm(ks[7], (DEPTH, D_ATT), 0.02),
        "w_out": nrm(ks[8], (DEPTH, D_MIX, D_MODEL), DEEPNORM_BETA * D_MIX ** -0.5),
        "ln1_g": 1.0 + nrm(ks[9], (DEPTH, D_MODEL), 0.02),
        "ln1_b": nrm(ks[10], (DEPTH, D_MODEL), 0.01),
        "w_router": nrm(ks[11], (DEPTH, D_MODEL, N_EXPERTS), D_MODEL ** -0.5),
        "router_bias": nrm(ks[12], (DEPTH, N_EXPERTS), 0.01),
        "w_gate": nrm(ks[13], (DEPTH, N_EXPERTS, D_MODEL, D_EXPERT), D_MODEL ** -0.5),
        "w_up": nrm(ks[14], (DEPTH, N_EXPERTS, D_MODEL, D_EXPERT), DEEPNORM_BETA * D_MODEL ** -0.5),
        "w_down": nrm(ks[15], (DEPTH, N_EXPERTS, D_EXPERT, D_MODEL), DEEPNORM_BETA * D_EXPERT ** -0.5),
        "ws_gate": nrm(ks[16], (DEPTH, D_MODEL, D_EXPERT), D_MODEL ** -0.5),
        "ws_up": nrm(ks[17], (DEPTH, D_MODEL, D_EXPERT), DEEPNORM_BETA * D_MODEL ** -0.5),
        "ws_down": nrm(ks[18], (DEPTH, D_EXPERT, D_MODEL), DEEPNORM_BETA * D_EXPERT ** -0.5),
        "ln2_g": 1.0 + nrm(ks[19], (DEPTH, D_MODEL), 0.02),
        "ln2_b": nrm(ks[20], (DEPTH, D_MODEL), 0.01),
    }


def reference(x, c, w_mod, b_mod, w_in, conv_w, g_conv, g_att, w_out, ln1_g, ln1_b,
              w_router, router_bias, w_gate, w_up, w_down, ws_gate, ws_up, ws_down,
              ln2_g, ln2_b):
    for l in range(DEPTH):
        mod = (c @ w_mod[l] + b_mod[l])[:, None, :]
        sh1, sc1, g1, sh2, sc2, g2 = jnp.split(mod, N_MOD, axis=-1)
        u = x * (1.0 + sc1) + sh1
        mix = hybrid_mixer(u, w_in[l], conv_w[l], g_conv[l], g_att[l], w_out[l])
        x = layer_norm(DEEPNORM_ALPHA * x + (1.0 + g1) * mix, ln1_g[l], ln1_b[l])
        u = x * (1.0 + sc2) + sh2
        ffn = moe_ffn(u, w_router[l], router_bias[l], w_gate[l], w_up[l], w_down[l],
                      ws_gate[l], ws_up[l], ws_down[l])
        x = layer_norm(DEEPNORM_ALPHA * x + (1.0 + g2) * ffn, ln2_g[l], ln2_b[l])
    return x
```

```python
import contextlib
import numpy as np
import ml_dtypes
import concourse.bass as bass
import concourse.mybir as mybir
from concourse.bass_utils import run_bass_kernel_spmd

F32 = mybir.dt.float32
BF16 = mybir.dt.bfloat16
AF = mybir.ActivationFunctionType
ALU = mybir.AluOpType
AX = mybir.AxisListType

NCORES = 8
D = 2048
SEQ = 8192
TPC = SEQ // NCORES
DEPTH = 4
KC = D // 128
NE = 64
DE = 384
ALPHA = (2 * DEPTH) ** 0.25
LN_EPS = 1e-5
RMS_EPS = 1e-6


class Sched:
    ENGS = ("tensor", "vector", "scalar", "gpsimd", "sync")

    def __init__(self, nc):
        self.nc = nc
        self.ops = []

    def op(self, eng, fn, reads=(), writes=()):
        self.ops.append(dict(eng=eng, fn=fn, reads=tuple(reads), writes=tuple(writes), dma=None, sig=False))

    def dma(self, queue, fn, reads=(), writes=(), sem=None):
        assert sem is not None
        self.ops.append(dict(eng=queue, fn=fn, reads=tuple(reads), writes=tuple(writes), dma=sem, sig=True))

    def build(self, stack):
        nc = self.nc
        ops = self.ops
        last_w = {}
        readers = {}
        for i, o in enumerate(ops):
            deps = set()
            for k in o["reads"]:
                if k in last_w:
                    deps.add(last_w[k])
            for k in o["writes"]:
                if k in last_w:
                    deps.add(last_w[k])
                deps.update(readers.get(k, ()))
            deps.discard(i)
            o["deps"] = deps
            for k in o["reads"]:
                readers.setdefault(k, []).append(i)
            for k in o["writes"]:
                last_w[k] = i
                readers[k] = []
        for o in ops:
            for j in o["deps"]:
                pj = ops[j]
                if pj["dma"] is None:
                    if pj["eng"] == o["eng"] and pj["eng"] == "tensor":
                        continue
                    pj["sig"] = True
        eng_sem = {e: stack.enter_context(nc.semaphore("se_" + e)) for e in self.ENGS}
        dma_sem = {}
        for o in ops:
            if o["dma"] is not None and o["dma"] not in dma_sem:
                dma_sem[o["dma"]] = stack.enter_context(nc.semaphore("sd_" + str(o["dma"])))
        eng_cnt = {e: 0 for e in self.ENGS}
        dma_cnt = {k: 0 for k in dma_sem}
        per_eng = {e: [] for e in self.ENGS}
        for i, o in enumerate(ops):
            waits = {}
            for j in o["deps"]:
                pj = ops[j]
                if pj["dma"] is not None:
                    s = dma_sem[pj["dma"]]
                    v = dma_cnt[pj["dma"]]
                else:
                    if pj["eng"] == o["eng"] and pj["eng"] == "tensor":
                        continue
                    s = eng_sem[pj["eng"]]
                    v = pj["sigval"]
                key = id(s)
                if key not in waits or waits[key][1] < v:
                    waits[key] = (s, v)
            if o["dma"] is not None:
                dma_cnt[o["dma"]] += 16
                o["sigval"] = dma_cnt[o["dma"]]
            elif o["sig"]:
                eng_cnt[o["eng"]] += 1
                o["sigval"] = eng_cnt[o["eng"]]
            per_eng[o["eng"]].append((list(waits.values()), o))

        block = stack.enter_context(nc.Block())

        def emit(eng_name, eng):
            waited = {}
            for waits, o in per_eng[eng_name]:
                for s, v in waits:
                    if waited.get(id(s), 0) >= v:
                        continue
                    waited[id(s)] = v
                    eng.wait_ge(s, v)
                if o["fn"] is None:
                    continue
                inst = o["fn"](eng)
                if o["dma"] is not None:
                    inst.then_inc(dma_sem[o["dma"]], 16)
                elif o["sig"]:
                    inst.then_inc(eng_sem[eng_name], 1)

        @block.tensor
        def _(e):
            emit("tensor", e)

        @block.vector
        def _(e):
            emit("vector", e)

        @block.scalar
        def _(e):
            emit("scalar", e)

        @block.gpsimd
        def _(e):
            emit("gpsimd", e)

        @block.sync
        def _(e):
            emit("sync", e)


class Ctx:
    def __init__(self):
        self.nc = bass.Bass("TRN2", target_bir_lowering=False)
        self.S = Sched(self.nc)
        self.stack = contextlib.ExitStack()
        self.n = 0

    def sb(self, shape, dt, name=None):
        self.n += 1
        return self.stack.enter_context(self.nc.sbuf_tensor("s_" + (name or f"sb{self.n}"), list(shape), dt))

    def ps(self, name=None):
        self.n += 1
        return self.stack.enter_context(self.nc.psum_tensor("p_" + (name or f"ps{self.n}"), [128, 512], F32))

    def dram_in(self, name, shape, dt):
        return self.nc.dram_tensor(name, list(shape), dt, kind="ExternalInput").ap()

    def dram_out(self, name, shape, dt):
        return self.nc.dram_tensor(name, list(shape), dt, kind="ExternalOutput").ap()

    def finish(self, out_keys):
        self.S.op("sync", None, reads=out_keys)
        self.S.build(self.stack)
        self.stack.close()
        return self.nc


def build_p1():
    C = Ctx()
    nc, S = C.nc, C.S
    T = TPC
    xT = C.dram_in("xT", [D, T], F32)
    modT = C.dram_in("modT", [128, 96], F32)
    w_in = C.dram_in("w_in", [D, 6144], F32)
    o_b = C.dram_out("o_b", [1024, T], F32)
    o_vc = C.dram_out("o_vc", [1024, T], F32)
    o_q = C.dram_out("o_q", [1024, T], BF16)
    o_k = C.dram_out("o_k", [1024, T], BF16)
    o_v = C.dram_out("o_v", [T, 1024], BF16)

    mod = C.sb([128, 96], F32, "mod")
    opsc = C.sb([128, 16], F32, "opsc")
    uT = C.sb([128, KC, T], BF16, "uT")
    cT = C.sb([128, 8, T], F32, "cT")
    xs = [C.sb([128, T], F32, f"xs{i}") for i in range(2)]
    wst = [C.sb([128, KC, 512], F32, f"wst{i}") for i in range(2)]
    wbf = [C.sb([128, KC, 512], BF16, f"wbf{i}") for i in range(2)]
    stg = [C.sb([128, 512], F32, f"stg{i}") for i in range(4)]
    stgb = [C.sb([128, 512], BF16, f"stgb{i}") for i in range(4)]
    pss = [C.ps(f"ps{i}") for i in range(6)]

    S.dma("sync", lambda e: e.dma_start(out=mod[:], in_=modT), writes=["mod"], sem="mod")
    S.op("vector", lambda e: e.tensor_scalar_add(opsc[:], mod[:, 16:32], 1.0), reads=["mod"], writes=["opsc"])
    xTv = xT.rearrange("(c p) t -> c p t", p=128)
    for c in range(KC):
        sl = c % 2
        S.dma("sync", lambda e, c=c, sl=sl: e.dma_start(out=xs[sl][:], in_=xTv[c]), writes=[f"xs{sl}"], sem=f"xs{sl}")
        S.op("scalar", lambda e, c=c, sl=sl: e.activation(out=uT[:, c, :], in_=xs[sl][:], func=AF.Identity,
                                                         bias=mod[:, c:c + 1], scale=opsc[:, c:c + 1]),
             reads=[f"xs{sl}", "mod", "opsc"], writes=[f"uT{c}"])
    uT_keys = [f"uT{c}" for c in range(KC)]

    w_v = w_in.rearrange("(kc p) n -> p kc n", p=128)
    order = [4, 5, 0, 1, 2, 3, 6, 7, 8, 9, 10, 11]
    pcount = 0
    scount = 0
    out_keys = []
    for bi, nb in enumerate(order):
        sl = bi % 2
        for half in range(2):
            S.dma("sync" if half == 0 else "gpsimd",
                  lambda e, nb=nb, sl=sl, half=half: e.dma_start(
                      out=wst[sl][:, half * 8:(half + 1) * 8, :],
                      in_=w_v[:, half * 8:(half + 1) * 8, nb * 512:(nb + 1) * 512]),
                  writes=[f"wst{sl}_{half}"], sem=f"wst{sl}_{half}")
            S.op("vector" if half == 0 else "gpsimd",
                 lambda e, sl=sl, half=half: e.tensor_copy(out=wbf[sl][:, half * 8:(half + 1) * 8, :],
                                                          in_=wst[sl][:, half * 8:(half + 1) * 8, :]),
                 reads=[f"wst{sl}_{half}"], writes=[f"wbf{sl}_{half}"])
        wkeys = [f"wbf{sl}_0", f"wbf{sl}_1"]
        kind = nb // 2
        if kind < 5:
            for m in range(4):
                fch = (nb % 2) * 4 + m
                for nh in range(2):
                    p = pcount % 6
                    pcount += 1
                    for kc in range(KC):
                        S.op("tensor", lambda e, p=p, sl=sl, kc=kc, m=m, nh=nh: e.matmul(
                            pss[p][:], lhsT=wbf[sl][:, kc, m * 128:(m + 1) * 128],
                            rhs=uT[:, kc, nh * 512:(nh + 1) * 512], start=(kc == 0), stop=(kc == KC - 1)),
                            reads=wkeys + uT_keys, writes=[f"ps{p}"])
                    tsl = slice(nh * 512, (nh + 1) * 512)
                    if kind == 2:
                        S.op("scalar", lambda e, p=p, fch=fch, tsl=tsl: e.copy(out=cT[:, fch, tsl], in_=pss[p][:]),
                             reads=[f"ps{p}"], writes=[f"cT{fch}_{nh}"])
                        continue
                    s = scount % 4
                    scount += 1
                    if kind == 0:
                        S.op("vector", lambda e, p=p, s=s, fch=fch, tsl=tsl: e.tensor_tensor(
                            out=stg[s][:], in0=pss[p][:], in1=cT[:, fch, tsl], op=ALU.mult),
                            reads=[f"ps{p}", f"cT{fch}_{nh}"], writes=[f"stg{s}"])
                        dst = o_vc[fch * 128:(fch + 1) * 128, tsl]
                        src = stg[s]
                        skey = f"stg{s}"
                    elif kind == 1:
                        S.op("scalar", lambda e, p=p, s=s: e.copy(out=stg[s][:], in_=pss[p][:]),
                             reads=[f"ps{p}"], writes=[f"stg{s}"])
                        dst = o_b[fch * 128:(fch + 1) * 128, tsl]
                        src = stg[s]
                        skey = f"stg{s}"
                    elif kind == 3:
                        S.op("scalar", lambda e, p=p, s=s: e.mul(stgb[s][:], pss[p][:], 128.0 ** -0.5),
                             reads=[f"ps{p}"], writes=[f"stgb{s}"])
                        dst = o_q[fch * 128:(fch + 1) * 128, tsl]
                        src = stgb[s]
                        skey = f"stgb{s}"
                    else:
                        S.op("vector", lambda e, p=p, s=s: e.tensor_copy(out=stgb[s][:], in_=pss[p][:]),
                             reads=[f"ps{p}"], writes=[f"stgb{s}"])
                        dst = o_k[fch * 128:(fch + 1) * 128, tsl]
                        src = stgb[s]
                        skey = f"stgb{s}"
                    ok = f"out{len(out_keys)}"
                    out_keys.append(ok)
                    S.dma("sync", lambda e, dst=dst, src=src: e.dma_start(out=dst, in_=src[:]),
                          reads=[skey], writes=[ok], sem="st_" + skey)
        else:
            for tt in range(8):
                p = pcount % 6
                pcount += 1
                for kc in range(KC):
                    S.op("tensor", lambda e, p=p, sl=sl, kc=kc, tt=tt: e.matmul(
                        pss[p][:], lhsT=uT[:, kc, tt * 128:(tt + 1) * 128],
                        rhs=wbf[sl][:, kc, :], start=(kc == 0), stop=(kc == KC - 1)),
                        reads=wkeys + uT_keys, writes=[f"ps{p}"])
                s = scount % 4
                scount += 1
                eng = "vector" if tt % 2 == 0 else "scalar"
                if eng == "vector":
                    S.op("vector", lambda e, p=p, s=s: e.tensor_copy(out=stgb[s][:], in_=pss[p][:]),
                         reads=[f"ps{p}"], writes=[f"stgb{s}"])
                else:
                    S.op("scalar", lambda e, p=p, s=s: e.copy(out=stgb[s][:], in_=pss[p][:]),
                         reads=[f"ps{p}"], writes=[f"stgb{s}"])
                col = (nb % 2) * 512
                dst = o_v[tt * 128:(tt + 1) * 128, col:col + 512]
                ok = f"out{len(out_keys)}"
                out_keys.append(ok)
                S.dma("sync", lambda e, dst=dst, s=s: e.dma_start(out=dst, in_=stgb[s][:]),
                      reads=[f"stgb{s}"], writes=[ok], sem=f"st_stgb{s}")
    return C.finish(out_keys)


def mod_layout(mod_row):
    return np.ascontiguousarray(mod_row.reshape(96, 128).T)


def attn_consts():
    j = np.arange(128)
    negU = -(j[:, None] >= j[None, :]).astype(np.float32)
    negO = -np.ones((128, 128), np.float32)
    t = np.arange(512)
    msk = np.zeros((128, 4, 512), np.float32)
    for jj in range(4):
        msk[:, jj, :] = ((jj * 128 + j)[:, None] < t[None, :])
    bf = ml_dtypes.bfloat16
    return negU.astype(bf), negO.astype(bf), msk.astype(bf)


def build_p2():
    C = Ctx()
    nc, S = C.nc, C.S
    qT_d = C.dram_in("qT", [128, SEQ], BF16)
    kT_d = C.dram_in("kT", [128, SEQ], BF16)
    v_d = C.dram_in("v", [SEQ, 128], BF16)
    negU_d = C.dram_in("negU", [128, 128], BF16)
    negO_d = C.dram_in("negO", [128, 128], BF16)
    msk_d = C.dram_in("msk", [128, 4, 512], BF16)
    oT_d = C.dram_out("oT", [128, SEQ], F32)

    qT = C.sb([128, SEQ], BF16, "qT")
    kT = C.sb([128, SEQ], BF16, "kT")
    v = C.sb([128, 64, 128], BF16, "v")
    negU = C.sb([128, 128], BF16, "negU")
    negO = C.sb([128, 128], BF16, "negO")
    msk = C.sb([128, 4, 512], BF16, "msk")
    e_sb = [C.sb([128, 512], F32, f"e{i}") for i in range(2)]
    sp_sb = [C.sb([128, 512], BF16, f"sp{i}") for i in range(3)]
    S_sb = [C.sb([128, 512], BF16, f"S{i}") for i in range(3)]
    a_sb = [C.sb([128, 512], BF16, f"a{i}") for i in range(3)]
    o_sb = [C.sb([128, 512], F32, f"o{i}") for i in range(2)]
    psA = [C.ps(f"psA{i}") for i in range(2)]
    psB = [C.ps(f"psB{i}") for i in range(3)]
    psO = [C.ps(f"psO{i}") for i in range(2)]

    for q4 in range(4):
        sl = slice(q4 * 2048, (q4 + 1) * 2048)
        S.dma("sync", lambda e, sl=sl: e.dma_start(out=qT[:, sl], in_=qT_d[:, sl]), writes=[f"qT{q4}"], sem=f"qT{q4}")
        S.dma("gpsimd", lambda e, sl=sl: e.dma_start(out=kT[:, sl], in_=kT_d[:, sl]), writes=[f"kT{q4}"], sem=f"kT{q4}")
        S.dma("sync", lambda e, q4=q4: e.dma_start(
            out=v[:, q4 * 16:(q4 + 1) * 16, :],
            in_=v_d[q4 * 2048:(q4 + 1) * 2048, :].rearrange("(b p) d -> p b d", p=128)),
            writes=[f"v{q4}"], sem=f"v{q4}")
    S.dma("sync", lambda e: e.dma_start(out=negU[:], in_=negU_d), writes=["negU"], sem="negU")
    S.dma("sync", lambda e: e.dma_start(out=negO[:], in_=negO_d), writes=["negO"], sem="negO")
    S.dma("sync", lambda e: e.dma_start(out=msk[:], in_=msk_d), writes=["msk"], sem="msk")

    tiles = []
    for qg in range(16):
        for kb in range(4 * qg + 3, -1, -1):
            tiles.append((qg, kb))
    n = len(tiles)
    out_keys = []

    def stA(i):
        qg, kb = tiles[i]
        pa = i % 2
        S.op("tensor", lambda e: e.matmul(psA[pa][:], lhsT=kT[:, kb * 128:(kb + 1) * 128],
                                          rhs=qT[:, qg * 512:(qg + 1) * 512], start=True, stop=True),
             reads=[f"kT{kb // 16}", f"qT{qg // 4}"], writes=[f"psA{pa}"])

    def stB(i):
        qg, kb = tiles[i]
        pa = i % 2
        es = i % 2
        ss = i % 3
        S.op("scalar", lambda e: e.activation(out=e_sb[es][:], in_=psA[pa][:], func=AF.Exp),
             reads=[f"psA{pa}"], writes=[f"e{es}"])
        S.op("scalar", lambda e: e.activation(out=sp_sb[ss][:], in_=e_sb[es][:], func=AF.Ln, bias=1.0),
             reads=[f"e{es}"], writes=[f"sp{ss}"])
        j = kb - 4 * qg
        if j >= 0:
            S.op("vector", lambda e: e.tensor_tensor(out=sp_sb[ss][:], in0=sp_sb[ss][:], in1=msk[:, j, :], op=ALU.mult),
                 reads=[f"sp{ss}", "msk"], writes=[f"sp{ss}"])
        first = (kb == 4 * qg + 3)
        if first:
            S.op("gpsimd", lambda e: e.tensor_copy(out=S_sb[ss][:], in_=sp_sb[ss][:]),
                 reads=[f"sp{ss}"], writes=[f"S{ss}"])
        else:
            so = (i - 1) % 3
            S.op("gpsimd", lambda e: e.tensor_tensor(out=S_sb[ss][:], in0=S_sb[so][:], in1=sp_sb[ss][:], op=ALU.add),
                 reads=[f"sp{ss}", f"S{so}"], writes=[f"S{ss}"])

    def stC(i):
        qg, kb = tiles[i]
        pb = i % 3
        ss = i % 3
        first = (kb == 4 * qg + 3)
        S.op("tensor", lambda e: e.matmul(psB[pb][:], lhsT=kT[:, kb * 128:(kb + 1) * 128],
                                          rhs=qT[:, qg * 512:(qg + 1) * 512], start=True, stop=False),
             reads=[f"kT{kb // 16}", f"qT{qg // 4}"], writes=[f"psB{pb}"])
        S.op("tensor", lambda e: e.matmul(psB[pb][:], lhsT=negU[:], rhs=sp_sb[ss][:], start=False, stop=first),
             reads=["negU", f"sp{ss}"], writes=[f"psB{pb}"])
        if not first:
            so = (i - 1) % 3
            S.op("tensor", lambda e: e.matmul(psB[pb][:], lhsT=negO[:], rhs=S_sb[so][:], start=False, stop=True),
                 reads=["negO", f"S{so}"], writes=[f"psB{pb}"])

    def stD(i):
        qg, kb = tiles[i]
        pb = i % 3
        sa = i % 3
        S.op("scalar", lambda e: e.activation(out=a_sb[sa][:], in_=psB[pb][:], func=AF.Exp),
             reads=[f"psB{pb}"], writes=[f"a{sa}"])
        j = kb - 4 * qg
        if j >= 0:
            S.op("vector", lambda e: e.tensor_tensor(out=a_sb[sa][:], in0=a_sb[sa][:], in1=msk[:, j, :], op=ALU.mult),
                 reads=[f"a{sa}", "msk"], writes=[f"a{sa}"])

    def stE(i):
        qg, kb = tiles[i]
        sa = i % 3
        po = qg % 2
        S.op("tensor", lambda e: e.matmul(psO[po][:], lhsT=v[:, kb, :], rhs=a_sb[sa][:],
                                          start=(kb == 4 * qg + 3), stop=(kb == 0)),
             reads=[f"v{kb // 16}", f"a{sa}"], writes=[f"psO{po}"])
        if kb == 0:
            S.op("vector", lambda e: e.tensor_copy(out=o_sb[po][:], in_=psO[po][:]),
                 reads=[f"psO{po}"], writes=[f"o{po}"])
            ok = f"out{qg}"
            out_keys.append(ok)
            S.dma("sync", lambda e: e.dma_start(out=oT_d[:, qg * 512:(qg + 1) * 512], in_=o_sb[po][:]),
                  reads=[f"o{po}"], writes=[ok], sem=f"st_o{po}")

    for s in range(n + 4):
        if s < n:
            stA(s)
        if 0 <= s - 1 < n:
            stB(s - 1)
        if 0 <= s - 2 < n:
            stC(s - 2)
        if 0 <= s - 3 < n:
            stD(s - 3)
        if 0 <= s - 4 < n:
            stE(s - 4)
    return C.finish(out_keys)


def build_p0():
    C = Ctx()
    nc, S = C.nc, C.S
    NCOL = DEPTH * 6 * D // NCORES
    cT_d = C.dram_in("cT", [128, KC], F32)
    w_d = C.dram_in("wm", [D, NCOL], F32)
    b_d = C.dram_in("bm", [1, NCOL], F32)
    o_d = C.dram_out("mo", [1, NCOL], F32)
    cT = C.sb([128, KC], F32, "cT")
    bm = C.sb([1, NCOL], F32, "bm")
    om = C.sb([1, NCOL], F32, "om")
    wt = [C.sb([128, KC, 512], F32, f"wt{i}") for i in range(2)]
    P = [C.ps(f"P{i}") for i in range(2)]
    S.dma("sync", lambda e: e.dma_start(out=cT[:], in_=cT_d), writes=["cT"], sem="cT")
    S.dma("sync", lambda e: e.dma_start(out=bm[:], in_=b_d), writes=["bm"], sem="bm")
    wv = w_d.rearrange("(kc p) n -> p kc n", p=128)
    for nb in range(NCOL // 512):
        sl = nb % 2
        for hf in range(2):
            S.dma("sync" if hf == 0 else "gpsimd",
                  lambda e, nb=nb, sl=sl, hf=hf: e.dma_start(out=wt[sl][:, hf * 8:(hf + 1) * 8, :],
                                                            in_=wv[:, hf * 8:(hf + 1) * 8, nb * 512:(nb + 1) * 512]),
                  writes=[f"wt{sl}_{hf}"], sem=f"wt{sl}_{hf}")
        for kc in range(KC):
            S.op("tensor", lambda e, sl=sl, kc=kc: e.matmul(P[sl][0:1, :], lhsT=cT[:, kc:kc + 1], rhs=wt[sl][:, kc, :],
                                                           start=(kc == 0), stop=(kc == KC - 1)),
                 reads=["cT", f"wt{sl}_0", f"wt{sl}_1"], writes=[f"P{sl}"])
        S.op("vector", lambda e, sl=sl, nb=nb: e.tensor_tensor(out=om[0:1, nb * 512:(nb + 1) * 512], in0=P[sl][0:1, :],
                                                              in1=bm[0:1, nb * 512:(nb + 1) * 512], op=ALU.add),
             reads=[f"P{sl}", "bm"], writes=["om"])
    S.dma("sync", lambda e: e.dma_start(out=o_d, in_=om[:]), reads=["om"], writes=["out"], sem="out")
    return C.finish(["out"])


def build_p3(debug=False):
    C = Ctx()
    nc, S = C.nc, C.S
    T = TPC
    BIG = 1.0e9
    xT_d = C.dram_in("xT", [D, T], F32)
    modT_d = C.dram_in("modT", [128, 96], F32)
    oT_d = C.dram_in("oT", [1024, T], F32)
    b_d = C.dram_in("b", [1024, T], F32)
    vc_d = C.dram_in("vc", [1024, T + 2], F32)
    cw_d = C.dram_in("convw", [128, 24], F32)
    gca_d = C.dram_in("gca", [128, 16], F32)
    lnp_d = C.dram_in("lnp", [128, 64], F32)
    wout_d = C.dram_in("w_out", [D, D], F32)
    wr_d = C.dram_in("w_router", [D, NE], F32)
    rb_d = C.dram_in("rbias", [128, NE], F32)
    wg_d = C.dram_in("w_gate", [NE, D, DE], F32)
    wu_d = C.dram_in("w_up", [NE, D, DE], F32)
    wd_d = C.dram_in("w_down", [NE, DE, D], F32)
    wsg_d = C.dram_in("ws_gate", [D, DE], F32)
    wsu_d = C.dram_in("ws_up", [D, DE], F32)
    wsd_d = C.dram_in("ws_down", [DE, D], F32)
    ident_d = C.dram_in("ident", [128, 128], F32)
    xo_d = C.dram_out("xo", [D, T], F32)

    acc = C.sb([128, KC, T], F32, "acc")
    uT = C.sb([128, KC, T], BF16, "uT")
    W = [C.sb([128, 6144], BF16, f"W{i}") for i in range(6)]
    hT = [C.sb([128, 3, 512], BF16, f"hT{i}") for i in range(2)]
    scr = [C.sb([128, 1026], F32, f"scr{i}") for i in range(4)]
    sqb = [C.sb([128, T], BF16, f"sqb{i}") for i in range(2)]
    mod = C.sb([128, 96], F32, "mod")
    opm = C.sb([128, 96], F32, "opm")
    cw = C.sb([128, 24], F32, "cw")
    gca = C.sb([128, 16], F32, "gca")
    lnp = C.sb([128, 64], F32, "lnp")
    rb = C.sb([128, NE], F32, "rb")
    ident = C.sb([128, 128], F32, "ident")
    onesb = C.sb([128, 128], BF16, "onesb")
    onesf = C.sb([128, 128], F32, "onesf")
    ones1 = C.sb([128, 128], F32, "ones1")
    dg = [C.sb([128, 128], F32, f"dg{i}") for i in range(4)]
    wr = C.sb([128, KC, NE], BF16, "wr")
    G = C.sb([128, 8, NE], F32, "G")
    rt = {k: C.sb([128, NE], F32, "rt_" + k) for k in ("sc", "sel", "top", "selm", "em", "w")}
    rs = {k: C.sb([128, 8], F32, "rs_" + k) for k in ("gs", "srt", "gm", "pen", "t8", "ws", "ri")}
    P = [C.ps(f"P{i}") for i in range(8)]

    def ld(dst, src, key, q="sync"):
        S.dma(q, lambda e: e.dma_start(out=dst, in_=src), writes=[key], sem=key)

    ld(mod[:], modT_d, "mod")
    ld(cw[:], cw_d, "cw")
    ld(gca[:], gca_d, "gca")
    ld(lnp[:], lnp_d, "lnp")
    ld(rb[:], rb_d, "rb")
    ld(ident[:], ident_d, "ident")
    S.dma("gpsimd", lambda e: e.dma_start(out=wr[:], in_=wr_d.rearrange("(kc p) n -> p kc n", p=128)),
          writes=["wr"], sem="wr")
    S.op("vector", lambda e: e.tensor_scalar_add(opm[:], mod[:], 1.0), reads=["mod"], writes=["opm"])
    S.op("vector", lambda e: e.memset(onesb[:], 1.0), writes=["onesb"])
    S.op("vector", lambda e: e.memset(onesf[:], 1.0 / D), writes=["onesf"])
    S.op("vector", lambda e: e.memset(ones1[:], 1.0), writes=["ones1"])

    halves = [slice(0, 512), slice(512, 1024)]

    def acck(c, h=None):
        return [f"acc{c}_{h}"] if h is not None else [f"acc{c}_0", f"acc{c}_1"]

    def rms_group(c0, prep, pb):
        for c in range(8):
            cc = c0 + c
            prep(c, cc)
            sq = sqb[cc % 2]
            S.op("scalar", lambda e, cc=cc, sq=sq: e.activation(out=sq[:], in_=acc[:, cc, :], func=AF.Square),
                 reads=acck(cc), writes=[f"sqb{cc % 2}"])
            for h in range(2):
                S.op("tensor", lambda e, h=h, sq=sq, c=c: e.matmul(P[pb + h][:], lhsT=onesb[:], rhs=sq[:, halves[h]],
                                                                  start=(c == 0), stop=(c == 7)),
                     reads=["onesb", f"sqb{cc % 2}"], writes=[f"P{pb + h}"])
        rstd = scr[3]
        for h in range(2):
            S.op("scalar", lambda e, h=h: e.activation(out=rstd[:, halves[h]], in_=P[pb + h][:], func=AF.Sqrt,
                                                      bias=RMS_EPS, scale=1.0 / 1024.0),
                 reads=[f"P{pb + h}"], writes=["scr3"])
        S.op("vector", lambda e: e.reciprocal(out=rstd[:, 0:T], in_=rstd[:, 0:T]), reads=["scr3"], writes=["scr3"])
        for c in range(8):
            cc = c0 + c
            S.op("vector", lambda e, cc=cc: e.scalar_tensor_tensor(out=uT[:, cc, :], in0=acc[:, cc, :],
                                                                   scalar=gca[:, cc:cc + 1], in1=rstd[:, 0:T],
                                                                   op0=ALU.mult, op1=ALU.mult),
                 reads=acck(cc) + ["gca", "scr3"], writes=[f"uT{cc}"])

    vc_v = vc_d.rearrange("(c p) t -> c p t", p=128)
    b_v = b_d.rearrange("(c p) t -> c p t", p=128)
    o_v = oT_d.rearrange("(c p) t -> c p t", p=128)

    def prep_conv(c, cc):
        sv = scr[c % 2]
        sbb = scr[2]
        S.dma("sync", lambda e: e.dma_start(out=sv[:], in_=vc_v[c]), writes=[f"scr{c % 2}"], sem=f"scr{c % 2}")
        S.dma("sync", lambda e: e.dma_start(out=sbb[:, 0:T], in_=b_v[c]), writes=["scr2"], sem="scr2")
        S.op("vector", lambda e: e.tensor_scalar(out=acc[:, cc, :], in0=sv[:, 0:T], scalar1=cw[:, 3 * c:3 * c + 1],
                                                 scalar2=None, op0=ALU.mult),
             reads=[f"scr{c % 2}", "cw"], writes=acck(cc))
        for k in (1, 2):
            S.op("vector", lambda e, k=k: e.scalar_tensor_tensor(out=acc[:, cc, :], in0=sv[:, k:k + T],
                                                                 scalar=cw[:, 3 * c + k:3 * c + k + 1],
                                                                 in1=acc[:, cc, :], op0=ALU.mult, op1=ALU.add),
                 reads=[f"scr{c % 2}", "cw"] + acck(cc), writes=acck(cc))
        S.op("vector", lambda e: e.tensor_tensor(out=acc[:, cc, :], in0=acc[:, cc, :], in1=sbb[:, 0:T], op=ALU.mult),
             reads=["scr2"] + acck(cc), writes=acck(cc))

    def prep_att(c, cc):
        S.dma("sync", lambda e: e.dma_start(out=acc[:, cc, :], in_=o_v[c]), writes=acck(cc), sem=f"ld_acc{cc}")

    rms_group(0, prep_conv, 0)
    rms_group(8, prep_att, 2)

    wo_v = wout_d.rearrange("(kc p) n -> p kc n", p=128)
    xT_v = xT_d.rearrange("(c p) t -> c p t", p=128)
    uT_keys = [f"uT{c}" for c in range(KC)]
    pc = 0
    for blk in range(8):
        ws = blk % 6
        wv = W[ws][:, 0:4096].rearrange("p (a b) -> p a b", a=KC)
        S.dma("gpsimd", lambda e, wv=wv, blk=blk: e.dma_start(out=wv, in_=wo_v[:, :, blk * 256:(blk + 1) * 256]),
              writes=[f"W{ws}"], sem=f"W{ws}")
        for m in range(2):
            dc = blk * 2 + m
            for h in range(2):
                p = 4 + pc % 4
                pc += 1
                for kc in range(KC):
                    S.op("tensor", lambda e, p=p, wv=wv, kc=kc, m=m, h=h: e.matmul(
                        P[p][:], lhsT=wv[:, kc, m * 128:(m + 1) * 128], rhs=uT[:, kc, halves[h]],
                        start=(kc == 0), stop=(kc == KC - 1)),
                        reads=[f"W{ws}"] + uT_keys, writes=[f"P{p}"])
                xs = scr[pc % 2]
                xk = f"scr{pc % 2}"
                S.dma("sync", lambda e, xs=xs, dc=dc, h=h: e.dma_start(out=xs[:, 0:512], in_=xT_v[dc][:, halves[h]]),
                      writes=[xk], sem=xk)
                S.op("scalar", lambda e, xs=xs: e.mul(xs[:, 0:512], xs[:, 0:512], ALPHA), reads=[xk], writes=[xk])
                S.op("vector", lambda e, p=p, xs=xs, dc=dc, h=h: e.scalar_tensor_tensor(
                    out=acc[:, dc, halves[h]], in0=P[p][:], scalar=opm[:, 32 + dc:33 + dc], in1=xs[:, 0:512],
                    op0=ALU.mult, op1=ALU.add),
                    reads=[f"P{p}", "opm", xk], writes=acck(dc, h))

    def layer_norm(goff, boff, post):
        for c in range(KC):
            sq = scr[c % 2]
            S.op("scalar", lambda e, c=c, sq=sq: e.activation(out=sq[:, 0:T], in_=acc[:, c, :], func=AF.Square),
                 reads=acck(c), writes=[f"scr{c % 2}"])
            for h in range(2):
                S.op("tensor", lambda e, c=c, h=h: e.matmul(P[h][:], lhsT=onesf[:], rhs=acc[:, c, halves[h]],
                                                           start=(c == 0), stop=(c == KC - 1)),
                     reads=["onesf"] + acck(c, h), writes=[f"P{h}"])
                S.op("tensor", lambda e, c=c, h=h, sq=sq: e.matmul(P[2 + h][:], lhsT=onesf[:], rhs=sq[:, halves[h]],
                                                                  start=(c == 0), stop=(c == KC - 1)),
                     reads=["onesf", f"scr{c % 2}"], writes=[f"P{2 + h}"])
        mean, rstd = scr[2], scr[3]
        for h in range(2):
            S.op("scalar", lambda e, h=h: e.copy(out=mean[:, halves[h]], in_=P[h][:]), reads=[f"P{h}"], writes=["scr2"])
        S.op("vector", lambda e: e.tensor_tensor(out=rstd[:, 0:T], in0=mean[:, 0:T], in1=mean[:, 0:T], op=ALU.mult),
             reads=["scr2"], writes=["scr3"])
        for h in range(2):
            S.op("vector", lambda e, h=h: e.tensor_tensor(out=rstd[:, halves[h]], in0=P[2 + h][:], in1=rstd[:, halves[h]],
                                                         op=ALU.subtract),
                 reads=[f"P{2 + h}", "scr3"], writes=["scr3"])
        S.op("scalar", lambda e: e.activation(out=rstd[:, 0:T], in_=rstd[:, 0:T], func=AF.Sqrt, bias=LN_EPS),
             reads=["scr3"], writes=["scr3"])
        S.op("vector", lambda e: e.reciprocal(out=rstd[:, 0:T], in_=rstd[:, 0:T]), reads=["scr3"], writes=["scr3"])
        for c in range(KC):
            S.op("vector", lambda e, c=c: e.tensor_tensor(out=acc[:, c, :], in0=acc[:, c, :], in1=mean[:, 0:T],
                                                         op=ALU.subtract),
                 reads=acck(c) + ["scr2"], writes=acck(c))
            S.op("gpsimd", lambda e, c=c: e.tensor_tensor(out=acc[:, c, :], in0=acc[:, c, :], in1=rstd[:, 0:T],
                                                         op=ALU.mult),
                 reads=acck(c) + ["scr3"], writes=acck(c))
            S.op("scalar", lambda e, c=c: e.activation(out=acc[:, c, :], in_=acc[:, c, :], func=AF.Identity,
                                                      bias=lnp[:, boff + c:boff + c + 1],
                                                      scale=lnp[:, goff + c:goff + c + 1]),
                 reads=acck(c) + ["lnp"], writes=acck(c))
            post(c)

    def post_ln1(c):
        S.op("scalar", lambda e: e.activation(out=uT[:, c, :], in_=acc[:, c, :], func=AF.Identity,
                                              bias=mod[:, 48 + c:49 + c], scale=opm[:, 64 + c:65 + c]),
             reads=acck(c) + ["mod", "opm"], writes=[f"uT{c}"])
        S.op("scalar", lambda e: e.mul(acc[:, c, :], acc[:, c, :], ALPHA), reads=acck(c), writes=acck(c))

    layer_norm(0, 16, post_ln1)

    for tt in range(8):
        for kc in range(KC):
            S.op("tensor", lambda e, tt=tt, kc=kc: e.matmul(P[0][:, tt * NE:(tt + 1) * NE],
                                                           lhsT=uT[:, kc, tt * 128:(tt + 1) * 128], rhs=wr[:, kc, :],
                                                           start=(kc == 0), stop=(kc == KC - 1)),
                 reads=["wr"] + uT_keys, writes=["P0"])
    RK = ["rt"]
    for tt in range(8):
        def V(fn, rd=(), wr_=()):
            S.op("vector", fn, reads=RK + list(rd), writes=RK + list(wr_))
        S.op("scalar", lambda e, tt=tt: e.activation(out=rt["sc"][:], in_=P[0][:, tt * NE:(tt + 1) * NE], func=AF.Sigmoid),
             reads=["P0"] + RK, writes=RK)
        V(lambda e: e.tensor_tensor(out=rt["sel"][:], in0=rt["sc"][:], in1=rb[:], op=ALU.add), rd=["rb"])
        for g in range(8):
            V(lambda e, g=g: e.max(out=rt["top"][:, g * 8:(g + 1) * 8], in_=rt["sel"][:, g * 8:(g + 1) * 8]))
        topv = rt["top"][:].rearrange("p (g k) -> p g k", k=8)
        V(lambda e: e.tensor_tensor(out=rs["gs"][:], in0=topv[:, :, 0], in1=topv[:, :, 1], op=ALU.add))
        V(lambda e: e.max(out=rs["srt"][:], in_=rs["gs"][:]))
        V(lambda e: e.tensor_scalar(out=rs["gm"][:], in0=rs["gs"][:], scalar1=rs["srt"][:, 3:4], scalar2=None, op0=ALU.is_ge))
        V(lambda e: e.tensor_scalar(out=rs["pen"][:], in0=rs["gm"][:], scalar1=1.0, scalar2=BIG, op0=ALU.subtract, op1=ALU.mult))
        for g in range(8):
            V(lambda e, g=g: e.tensor_scalar(out=rt["selm"][:, g * 8:(g + 1) * 8], in0=rt["sel"][:, g * 8:(g + 1) * 8],
                                             scalar1=rs["pen"][:, g:g + 1], scalar2=None, op0=ALU.add))
        V(lambda e: e.max(out=rs["t8"][:], in_=rt["selm"][:]))
        V(lambda e: e.tensor_scalar(out=rt["em"][:], in0=rt["selm"][:], scalar1=rs["t8"][:, 7:8], scalar2=None, op0=ALU.is_ge))
        V(lambda e: e.tensor_tensor(out=rt["w"][:], in0=rt["sc"][:], in1=rt["em"][:], op=ALU.mult))
        V(lambda e: e.tensor_reduce(out=rs["ws"][:, 0:1], in_=rt["w"][:], axis=AX.X, op=ALU.add))
        V(lambda e: e.reciprocal(out=rs["ri"][:, 0:1], in_=rs["ws"][:, 0:1]))
        V(lambda e, tt=tt: e.tensor_scalar(out=G[:, tt, :], in0=rt["w"][:], scalar1=rs["ri"][:, 0:1], scalar2=2.5,
                                           op0=ALU.mult, op1=ALU.mult), wr_=[f"G{tt}"])

    dbg_keys = []
    if debug:
        dG = C.dram_out("dbgG", [128, 8 * NE], F32)
        dU = C.dram_out("dbgU", [128, KC * T], BF16)
        S.dma("sync", lambda e: e.dma_start(out=dG, in_=G[:].rearrange("p a b -> p (a b)")), reads=[f"G{t}" for t in range(8)],
              writes=["dbgG"], sem="dbgG")
        S.dma("sync", lambda e: e.dma_start(out=dU, in_=uT[:].rearrange("p a b -> p (a b)")), reads=uT_keys,
              writes=["dbgU"], sem="dbgU")
        dbg_keys = ["dbgG", "dbgU"]
    units = [(ei, h) for ei in range(NE + 1) for h in range(2)]

    def load_expert(ei):
        base = (ei % 2) * 3
        if ei < NE:
            g_src, u_src, d_src = wg_d[ei], wu_d[ei], wd_d[ei]
        else:
            g_src, u_src, d_src = wsg_d, wsu_d, wsd_d
        for j, (src, pat, a) in enumerate(((g_src, "(kc p) f -> p kc f", KC), (u_src, "(kc p) f -> p kc f", KC),
                                           (d_src, "(fc p) d -> p fc d", 3))):
            dst = W[base + j][:].rearrange("p (a b) -> p a b", a=a)
            S.dma("gpsimd", lambda e, dst=dst, src=src, pat=pat: e.dma_start(out=dst, in_=src.rearrange(pat, p=128)),
                  writes=[f"W{base + j}"], sem=f"W{base + j}")

    cnt = {"gu": 0, "d": 0, "dg": 0}

    def gate_up(ui):
        ei, h = units[ui]
        base = (ei % 2) * 3
        wg = W[base][:].rearrange("p (a b) -> p a b", a=KC)
        wu = W[base + 1][:].rearrange("p (a b) -> p a b", a=KC)
        hs = ui % 2
        gbs = scr[2 + ui % 2]
        gk = f"scr{2 + ui % 2}"
        if ei < NE:
            for j in range(4):
                tt = h * 4 + j
                ds = cnt["dg"] % 4
                cnt["dg"] += 1
                S.op("vector", lambda e, ds=ds, tt=tt: e.tensor_scalar(out=dg[ds][:], in0=ident[:],
                                                                      scalar1=G[:, tt, ei:ei + 1], scalar2=None,
                                                                      op0=ALU.mult),
                     reads=[f"G{tt}", "ident"], writes=[f"dg{ds}"])
                S.op("tensor", lambda e, j=j, ds=ds: e.matmul(P[7][:, j * 128:(j + 1) * 128],
                                                             lhsT=ones1[:], rhs=dg[ds][:], start=True, stop=True),
                     reads=[f"dg{ds}", "ones1"], writes=["P7"])
            S.op("scalar", lambda e: e.copy(out=gbs[:, 0:512], in_=P[7][:]), reads=["P7"], writes=[gk])
        for fc in range(3):
            k = cnt["gu"]
            cnt["gu"] += 1
            pg, pu = k % 2, 2 + k % 2
            sg = scr[k % 2]
            sk = f"scr{k % 2}"
            for (pp, wv) in ((pg, wg), (pu, wu)):
                for kc in range(KC):
                    S.op("tensor", lambda e, pp=pp, wv=wv, kc=kc, fc=fc: e.matmul(
                        P[pp][:], lhsT=wv[:, kc, fc * 128:(fc + 1) * 128], rhs=uT[:, kc, halves[h]],
                        start=(kc == 0), stop=(kc == KC - 1)),
                        reads=[f"W{base}", f"W{base + 1}"] + uT_keys, writes=[f"P{pp}"])
            S.op("scalar", lambda e, pg=pg, sg=sg: e.activation(out=sg[:, 0:512], in_=P[pg][:], func=AF.Silu),
                 reads=[f"P{pg}"], writes=[sk])
            if ei < NE:
                S.op("vector", lambda e, pu=pu, sg=sg: e.tensor_tensor(out=sg[:, 0:512], in0=P[pu][:], in1=sg[:, 0:512],
                                                                      op=ALU.mult),
                     reads=[f"P{pu}", sk], writes=[sk])
                S.op("vector", lambda e, sg=sg, fc=fc: e.tensor_tensor(out=hT[hs][:, fc, :], in0=sg[:, 0:512],
                                                                      in1=gbs[:, 0:512], op=ALU.mult),
                     reads=[sk, gk], writes=[f"hT{hs}_{fc}"])
            else:
                S.op("vector", lambda e, pu=pu, sg=sg, fc=fc: e.tensor_tensor(out=hT[hs][:, fc, :], in0=P[pu][:],
                                                                             in1=sg[:, 0:512], op=ALU.mult),
                     reads=[f"P{pu}", sk], writes=[f"hT{hs}_{fc}"])

    def down(ui):
        ei, h = units[ui]
        base = (ei % 2) * 3
        wd = W[base + 2][:].rearrange("p (a b) -> p a b", a=3)
        hs = ui % 2
        for dc in range(KC):
            k = cnt["d"]
            cnt["d"] += 1
            p = 4 + k % 3
            for fc in range(3):
                S.op("tensor", lambda e, p=p, fc=fc, dc=dc: e.matmul(
                    P[p][:], lhsT=wd[:, fc, dc * 128:(dc + 1) * 128], rhs=hT[hs][:, fc, :],
                    start=(fc == 0), stop=(fc == 2)),
                    reads=[f"W{base + 2}", f"hT{hs}_{fc}"], writes=[f"P{p}"])
            S.op("vector", lambda e, p=p, dc=dc: e.scalar_tensor_tensor(
                out=acc[:, dc, halves[h]], in0=P[p][:], scalar=opm[:, 80 + dc:81 + dc], in1=acc[:, dc, halves[h]],
                op0=ALU.mult, op1=ALU.add),
                reads=[f"P{p}", "opm"] + acck(dc, h), writes=acck(dc, h))

    load_expert(0)
    for ui in range(len(units) + 1):
        if ui < len(units):
            gate_up(ui)
        if ui >= 1:
            down(ui - 1)
        if ui < len(units):
            ei, h = units[ui]
            if h == 0 and ei + 1 <= NE:
                load_expert(ei + 1)

    out_keys = []
    xo_v = xo_d.rearrange("(c p) t -> c p t", p=128)

    def post_ln2(c):
        ok = f"out{c}"
        out_keys.append(ok)
        S.dma("sync", lambda e: e.dma_start(out=xo_v[c], in_=acc[:, c, :]), reads=acck(c), writes=[ok], sem="out")

    layer_norm(32, 48, post_ln2)
    return C.finish(out_keys + dbg_keys)


_CACHE = {}


def _prog(name, fn):
    if name not in _CACHE:
        _CACHE[name] = fn()
    return _CACHE[name]


def _run(nc, in_maps):
    return run_bass_kernel_spmd(nc, in_maps, core_ids=list(range(NCORES))).results


def _cols(v):
    return np.ascontiguousarray(np.asarray(v, np.float32).reshape(-1, 128).T)


def kernel(x, c, w_mod, b_mod, w_in, conv_w, g_conv, g_att, w_out, ln1_g, ln1_b,
           w_router, router_bias, w_gate, w_up, w_down, ws_gate, ws_up, ws_down,
           ln2_g, ln2_b, depth=DEPTH):
    f32 = np.float32
    x = np.asarray(x, f32)
    wm = np.asarray(w_mod, f32)
    bm = np.asarray(b_mod, f32)
    ncol = DEPTH * 6 * D // NCORES
    maps = []
    for i in range(NCORES):
        l, hf = divmod(i, 2)
        maps.append({"cT": _cols(np.asarray(c, f32)[0]),
                     "wm": np.ascontiguousarray(wm[l][:, hf * ncol:(hf + 1) * ncol]),
                     "bm": np.ascontiguousarray(bm[l][None, hf * ncol:(hf + 1) * ncol])})
    r0 = _run(_prog("p0", build_p0), maps)
    mod_all = np.concatenate([r["mo"][0] for r in r0]).reshape(DEPTH, 6 * D)

    negU, negO, msk = attn_consts()
    ident = np.eye(128, dtype=f32)
    xT = [np.ascontiguousarray(x[0, i * TPC:(i + 1) * TPC].T) for i in range(NCORES)]
    for l in range(depth):
        modT = mod_layout(mod_all[l])
        wl_in = np.asarray(w_in[l], f32)
        r1 = _run(_prog("p1", build_p1), [{"xT": xT[i], "modT": modT, "w_in": wl_in} for i in range(NCORES)])
        qT = np.concatenate([r["o_q"] for r in r1], axis=1)
        kT = np.concatenate([r["o_k"] for r in r1], axis=1)
        vv = np.concatenate([r["o_v"] for r in r1], axis=0)
        maps = []
        for h in range(NCORES):
            hs = slice(h * 128, (h + 1) * 128)
            maps.append({"qT": np.ascontiguousarray(qT[hs]), "kT": np.ascontiguousarray(kT[hs]),
                         "v": np.ascontiguousarray(vv[:, hs]), "negU": negU, "negO": negO, "msk": msk})
        r2 = _run(_prog("p2", build_p2), maps)
        oT = np.concatenate([r["oT"] for r in r2], axis=0)
        vc_all = np.concatenate([r["o_vc"] for r in r1], axis=1)
        vc_pad = np.concatenate([np.zeros((1024, 2), f32), vc_all], axis=1)
        cw = np.ascontiguousarray(np.asarray(conv_w[l], f32).T.reshape(8, 128, 3).transpose(1, 0, 2).reshape(128, 24))
        gca = np.concatenate([_cols(g_conv[l]), _cols(g_att[l])], axis=1)
        lnp = np.concatenate([_cols(ln1_g[l]), _cols(ln1_b[l]), _cols(ln2_g[l]), _cols(ln2_b[l])], axis=1)
        rbias = np.ascontiguousarray(np.broadcast_to(np.asarray(router_bias[l], f32)[None, :], (128, NE)))
        shared = {"modT": modT, "convw": cw, "gca": np.ascontiguousarray(gca), "lnp": np.ascontiguousarray(lnp),
                  "w_out": np.asarray(w_out[l], f32), "w_router": np.asarray(w_router[l], f32), "rbias": rbias,
                  "w_gate": np.asarray(w_gate[l], f32), "w_up": np.asarray(w_up[l], f32),
                  "w_down": np.asarray(w_down[l], f32), "ws_gate": np.asarray(ws_gate[l], f32),
                  "ws_up": np.asarray(ws_up[l], f32), "ws_down": np.asarray(ws_down[l], f32), "ident": ident}
        maps = []
        for i in range(NCORES):
            ts = slice(i * TPC, (i + 1) * TPC)
            m = dict(shared)
            m.update({"xT": xT[i], "oT": np.ascontiguousarray(oT[:, ts]), "b": r1[i]["o_b"],
                      "vc": np.ascontiguousarray(vc_pad[:, i * TPC:(i + 1) * TPC + 2])})
            maps.append(m)
        r3 = _run(_prog("p3", build_p3), maps)
        xT = [r["xo"] for r in r3]
    out = np.concatenate([t.T for t in xT], axis=0)[None]
    return np.ascontiguousarray(out.astype(f32))
```
